# Optimizing a Trainium2 kernel written in Bass

```python
import jax
import jax.numpy as jnp
from jax import lax
import numpy as np

D_MODEL = 1024
BATCH = 4
SEQ = 8192
DEPTH = 4

GRID_W = 64
CTX_LEN = 256
N_BRANCH = 4
BRANCH_W = D_MODEL // 2
CONV_W = 3
MLSTM_HEADS = 4
MLSTM_HD = BRANCH_W // MLSTM_HEADS
MLSTM_CHUNK = 64
POOL_WINDOWS = (2, 4, 8, 16)
POOL_GW = BRANCH_W // len(POOL_WINDOWS)
ATT_HEAD_DIM = 64
ATT_Q_HEADS = BRANCH_W // ATT_HEAD_DIM
ATT_KV_HEADS = 2
ATT_WINDOW = 128
ATT_BLOCK = 128
ROPE_BASE = 10000.0
SC_W = BRANCH_W
D_FF = 2816
EPS = 1e-6

SPLITS = (BRANCH_W, BRANCH_W, BRANCH_W, BRANCH_W, 4 * MLSTM_HEADS,
          BRANCH_W,
          ATT_Q_HEADS * ATT_HEAD_DIM, ATT_KV_HEADS * ATT_HEAD_DIM, ATT_KV_HEADS * ATT_HEAD_DIM,
          SC_W, SC_W, SC_W,
          N_BRANCH * D_MODEL)
IN_WIDTH = sum(SPLITS)

kernel_name = 'hybrid_mlstm_pool_swa_conv_diffusion_block'


def split_cols(p):
    idx, acc = [], 0
    for w in SPLITS[:-1]:
        acc += w
        idx.append(acc)
    return jnp.split(p, idx, axis=-1)


def rmsnorm(x, g):
    xf = x.astype(jnp.float32)
    y = xf * lax.rsqrt(jnp.mean(xf * xf, axis=-1, keepdims=True) + EPS)
    return (y * g.astype(jnp.float32)).astype(x.dtype)


def modulate(h, shift, scale):
    return h * (1 + scale) + shift


def dwconv3(x, w):
    xp = jnp.pad(x, ((0, 0), (1, 1), (0, 0)))
    return xp[:, :-2] * w[0] + xp[:, 1:-1] * w[1] + xp[:, 2:] * w[2]


def axial_rope_tables(rows):
    row = jnp.repeat(jnp.arange(rows), GRID_W)
    col = jnp.tile(jnp.arange(GRID_W), rows)
    n_freq = ATT_HEAD_DIM // 4
    inv_freq = ROPE_BASE ** (-jnp.arange(n_freq, dtype=jnp.float32) / n_freq)
    ang = jnp.stack([row, col], axis=-1).astype(jnp.float32)[:, :, None] * inv_freq
    return jnp.cos(ang), jnp.sin(ang)


def apply_axial_rope(x, cos, sin):
    B, S, H, dh = x.shape
    xf = x.astype(jnp.float32).reshape(B, S, H, 2, 2, dh // 4)
    x1, x2 = xf[..., 0, :], xf[..., 1, :]
    c, s = cos[:, None], sin[:, None]
    out = jnp.stack([x1 * c - x2 * s, x1 * s + x2 * c], axis=-2)
    return out.reshape(B, S, H, dh).astype(x.dtype)


def mlstm_chunkwise(q, k, v, ig, lf, state):
    B, H, T, Dh = q.shape
    nc = T // MLSTM_CHUNK

    def chunks(a):
        return jnp.moveaxis(a.reshape(B, H, nc, MLSTM_CHUNK, *a.shape[3:]), 2, 0)

    tri = jnp.tril(jnp.ones((MLSTM_CHUNK, MLSTM_CHUNK), dtype=bool))

    def step(carry, inp):
        C, n, m = carry
        qc, kc, vc, ic, fc = inp
        b = jnp.cumsum(fc, axis=-1)
        a = b + m[..., None]
        dmat = jnp.where(tri, b[..., :, None] - b[..., None, :] + ic[..., None, :], -jnp.inf)
        mt = jnp.maximum(a, dmat.max(-1))
        inter = jnp.exp(a - mt)
        s = jnp.einsum('bhtd,bhsd->bhts', qc, kc) * jnp.exp(dmat - mt[..., None])
        num = jnp.einsum('bhts,bhsd->bhtd', s, vc) + inter[..., None] * jnp.einsum('bhtk,bhkv->bhtv', qc, C)
        den = s.sum(-1) + inter * jnp.einsum('bhtk,bhk->bht', qc, n)
        h = num / jnp.maximum(jnp.abs(den), jnp.exp(-mt))[..., None]
        m_new = mt[..., -1]
        decay = jnp.exp(b[..., -1] + m - m_new)
        wk = jnp.exp(b[..., -1:] - b + ic - m_new[..., None])
        C_new = decay[..., None, None] * C + jnp.einsum('bhs,bhsk,bhsv->bhkv', wk, kc, vc)
        n_new = decay[..., None] * n + jnp.einsum('bhs,bhsk->bhk', wk, kc)
        return (C_new, n_new, m_new), h

    state, hs = lax.scan(step, state, tuple(chunks(a) for a in (q, k, v, ig, lf)))
    return jnp.moveaxis(hs, 0, 2).reshape(B, H, T, Dh), state


def mlstm_inputs(q, k, v, g, qk_conv, gate_bias):
    B, T, _ = q.shape
    H, Dh = MLSTM_HEADS, MLSTM_HD
    qk = jax.nn.silu(dwconv3(jnp.concatenate([q, k], axis=-1), qk_conv)).astype(jnp.float32)
    heads = lambda a: a.reshape(B, T, H, Dh).transpose(0, 2, 1, 3)
    qh = heads(qk[..., :BRANCH_W])
    kh = heads(qk[..., BRANCH_W:]) * Dh ** -0.5
    vh = heads(v.astype(jnp.float32))
    gp = (g.astype(jnp.float32).reshape(B, T, 4, H) + gate_bias.astype(jnp.float32)).transpose(2, 0, 3, 1)
    return qh, kh, vh, gp[0:2], jax.nn.log_sigmoid(gp[2:4])


def mlstm_bidir(xin, cin, ctx_out):
    qx, kx, vx, ix, fx = xin
    qc, kc, vc, ic, fc = cin
    B, H, _, Dh = qx.shape
    zero = (jnp.zeros((B, H, Dh, Dh), jnp.float32), jnp.zeros((B, H, Dh), jnp.float32), jnp.zeros((B, H), jnp.float32))
    rev = lambda a: jnp.flip(a, axis=2)
    hc_f, st_f = mlstm_chunkwise(qc, kc, vc, ic[0], fc[0], zero)
    hc_b, st_b = mlstm_chunkwise(rev(qc), rev(kc), rev(vc), rev(ic[1]), rev(fc[1]), zero)
    hx_f, _ = mlstm_chunkwise(qx, kx, vx, ix[0], fx[0], st_f)
    hx_b, _ = mlstm_chunkwise(rev(qx), rev(kx), rev(vx), rev(ix[1]), rev(fx[1]), st_b)
    merge_heads = lambda a: a.transpose(0, 2, 1, 3).reshape(a.shape[0], a.shape[2], -1)
    hx = merge_heads(hx_f + rev(hx_b))
    hc = merge_heads(hc_f + rev(hc_b)) if ctx_out else None
    return hx, hc


def multiscale_pool(p, w_pool, scale):
    B, T, _ = p.shape
    G = len(POOL_WINDOWS)
    pf = p.astype(jnp.float32).reshape(B, T, G, POOL_GW)
    cs = jnp.pad(jnp.cumsum(pf, axis=1), ((0, 0), (1, 0), (0, 0), (0, 0)))
    half = jnp.array(POOL_WINDOWS) // 2
    t = jnp.arange(T)[:, None]
    lo = jnp.clip(t - half, 0, T)
    hi = jnp.clip(t + half, 0, T)
    g = jnp.arange(G)[None, :]
    mean = (cs[:, hi, g] - cs[:, lo, g]) / (hi - lo).astype(jnp.float32)[..., None]
    y = jnp.einsum('btgc,gcd->btgd', mean - pf, w_pool.astype(jnp.float32))
    return (y.reshape(B, T, -1) * scale.astype(jnp.float32)).astype(p.dtype)


def window_attention(q, k, v, k_ctx, v_ctx, sink):
    B, S, G, R, dh = q.shape
    nb = S // ATT_BLOCK
    scale = dh ** -0.5
    qb = q.reshape(B, nb, ATT_BLOCK, G, R, dh)

    def band(a):
        ab = jnp.pad(a.reshape(B, nb, ATT_BLOCK, G, dh), ((0, 0), (1, 1), (0, 0), (0, 0), (0, 0)))
        return jnp.concatenate([ab[:, :-2], ab[:, 1:-1], ab[:, 2:]], axis=2)

    kb, vb = band(k), band(v)
    blk = jnp.arange(nb)[:, None]
    qpos = blk * ATT_BLOCK + jnp.arange(ATT_BLOCK)[None]
    kpos = (blk - 1) * ATT_BLOCK + jnp.arange(3 * ATT_BLOCK)[None]
    valid = (jnp.abs(qpos[:, :, None] - kpos[:, None, :]) <= ATT_WINDOW) & ((kpos >= 0) & (kpos < S))[:, None, :]
    s_loc = jnp.einsum('bnqgrd,bnkgd->bngrqk', qb, kb).astype(jnp.float32) * scale
    s_loc = jnp.where(valid[None, :, None, None], s_loc, -jnp.inf)
    s_ctx = jnp.einsum('bnqgrd,blgd->bngrql', qb, k_ctx).astype(jnp.float32) * scale
    sk = sink.astype(jnp.float32).reshape(1, 1, G, R, 1)
    m = jnp.maximum(jnp.maximum(s_loc.max(-1), s_ctx.max(-1)), sk)
    e_loc = jnp.exp(s_loc - m[..., None])
    e_ctx = jnp.exp(s_ctx - m[..., None])
    den = e_loc.sum(-1) + e_ctx.sum(-1) + jnp.exp(sk - m)
    o = (jnp.einsum('bngrqk,bnkgd->bnqgrd', e_loc, vb.astype(jnp.float32))
         + jnp.einsum('bngrql,blgd->bnqgrd', e_ctx, v_ctx.astype(jnp.float32)))
    o = o / jnp.moveaxis(den, -1, 2)[..., None]
    return o.reshape(B, S, G * R * dh).astype(q.dtype)


def context_attention(q, k, v, sink):
    B, L, G, R, dh = q.shape
    s = jnp.einsum('blgrd,bmgd->bgrlm', q, k).astype(jnp.float32) * dh ** -0.5
    sk = jnp.broadcast_to(sink.astype(jnp.float32).reshape(1, G, R, 1, 1), s.shape[:-1] + (1,))
    p = jax.nn.softmax(jnp.concatenate([sk, s], axis=-1), axis=-1)[..., 1:]
    o = jnp.einsum('bgrlm,bmgd->blgrd', p, v.astype(jnp.float32))
    return o.reshape(B, L, G * R * dh).astype(q.dtype)


def merge_branches(p, h_mlstm, y_att, pool_w, pool_scale, sconv_w, w_branch, w_out):
    B, T, _ = h_mlstm.shape
    ya = (jax.nn.sigmoid(p[3]) * h_mlstm).astype(p[3].dtype)
    yb = multiscale_pool(p[5], pool_w, pool_scale)
    yd = p[9] * dwconv3(p[10] * p[11], sconv_w)
    ys = jnp.stack([ya, yb, y_att, yd], axis=2)
    proj = jnp.einsum('btnw,nwd->btnd', ys, w_branch)
    gates = jax.nn.sigmoid(p[12].reshape(B, T, N_BRANCH, -1))
    return jnp.einsum('btnd,btnd->btd', gates, proj) @ w_out


def token_mixers(ux, uc, w_in, qk_conv, gate_bias, pool_w, pool_scale, sink, sconv_w, w_branch, w_out, rope, ctx_out):
    B, S, _ = ux.shape
    L = uc.shape[1]
    px = split_cols(ux @ w_in)
    pc = split_cols(uc @ w_in)
    hx, hc = mlstm_bidir(mlstm_inputs(px[0], px[1], px[2], px[4], qk_conv, gate_bias),
                         mlstm_inputs(pc[0], pc[1], pc[2], pc[4], qk_conv, gate_bias), ctx_out)
    G, R, dh = ATT_KV_HEADS, ATT_Q_HEADS // ATT_KV_HEADS, ATT_HEAD_DIM
    k_ctx = pc[7].reshape(B, L, G, dh)
    v_ctx = pc[8].reshape(B, L, G, dh)
    q_x = apply_axial_rope(px[6].reshape(B, S, G * R, dh), *rope).reshape(B, S, G, R, dh)
    k_x = apply_axial_rope(px[7].reshape(B, S, G, dh), *rope)
    att_x = window_attention(q_x, k_x, px[8].reshape(B, S, G, dh), k_ctx, v_ctx, sink)
    out_x = merge_branches(px, hx, att_x, pool_w, pool_scale, sconv_w, w_branch, w_out)
    if not ctx_out:
        return out_x, None
    att_c = context_attention(pc[6].reshape(B, L, G, R, dh), k_ctx, v_ctx, sink)
    out_c = merge_branches(pc, hc, att_c, pool_w, pool_scale, sconv_w, w_branch, w_out)
    return out_x, out_c


def conv_ffn(u, w_up, w_conv, w_down):
    a = dwconv3(u @ w_up, w_conv)
    gate, val = jnp.split(a, 2, axis=-1)
    return (jax.nn.silu(gate) * val) @ w_down


def setup_inputs(seed: int = 0) -> dict:
    key = jax.random.key(seed)
    ks = jax.random.split(key, 20)
    D = D_MODEL
    nrm = lambda k, shape, s: jax.random.normal(k, shape, jnp.float32) * s
    x = nrm(ks[0], (BATCH, SEQ, D), 1.0)
    c = nrm(ks[1], (BATCH, D), 1.0)
    ctx = nrm(ks[2], (BATCH, CTX_LEN, D), 1.0)
    c_ctx = nrm(ks[3], (D,), 1.0)
    w_mod = nrm(ks[4], (DEPTH, D, 6 * D), 0.5 * D ** -0.5)
    b_mod = nrm(ks[5], (DEPTH, 6 * D), 0.01)
    norm_g = 1.0 + nrm(ks[6], (DEPTH, 4, D), 0.05)
    w_in = nrm(ks[7], (DEPTH, D, IN_WIDTH), D ** -0.5)
    mlstm_qk_conv = nrm(ks[8], (DEPTH, CONV_W, 2 * BRANCH_W), CONV_W ** -0.5)
    in_bias = nrm(ks[9], (DEPTH, 2, MLSTM_HEADS), 0.1)
    f_bias = jnp.linspace(3.0, 6.0, MLSTM_HEADS)[None, None] + nrm(ks[10], (DEPTH, 2, MLSTM_HEADS), 0.1)
    mlstm_gate_bias = jnp.concatenate([in_bias, f_bias], axis=1)
    pool_w = nrm(ks[11], (DEPTH, len(POOL_WINDOWS), POOL_GW, POOL_GW), POOL_GW ** -0.5)
    pool_scale = 1.0 + nrm(ks[12], (DEPTH, BRANCH_W), 0.05)
    attn_sink = nrm(ks[13], (DEPTH, ATT_Q_HEADS), 0.5)
    sconv_w = nrm(ks[14], (DEPTH, CONV_W, SC_W), CONV_W ** -0.5)
    w_branch = nrm(ks[15], (DEPTH, N_BRANCH, BRANCH_W, D), BRANCH_W ** -0.5)
    w_out = nrm(ks[16], (DEPTH, D, D), D ** -0.5)
    ffn_up = nrm(ks[17], (DEPTH, D, 2 * D_FF), D ** -0.5)
    ffn_conv = nrm(ks[18], (DEPTH, CONV_W, 2 * D_FF), CONV_W ** -0.5)
    ffn_down = nrm(ks[19], (DEPTH, D_FF, D), D_FF ** -0.5)
    return {'x': x, 'c': c, 'ctx': ctx, 'c_ctx': c_ctx, 'w_mod': w_mod, 'b_mod': b_mod, 'norm_g': norm_g,
            'w_in': w_in, 'mlstm_qk_conv': mlstm_qk_conv, 'mlstm_gate_bias': mlstm_gate_bias,
            'pool_w': pool_w, 'pool_scale': pool_scale, 'attn_sink': attn_sink, 'sconv_w': sconv_w,
            'w_branch': w_branch, 'w_out': w_out, 'ffn_up': ffn_up, 'ffn_conv': ffn_conv, 'ffn_down': ffn_down}


def reference(x, c, ctx, c_ctx, w_mod, b_mod, norm_g, w_in, mlstm_qk_conv, mlstm_gate_bias, pool_w, pool_scale,
              attn_sink, sconv_w, w_branch, w_out, ffn_up, ffn_conv, ffn_down):
    B, S, D = x.shape
    rows = S // GRID_W
    rope = axial_rope_tables(rows)
    sc_x = jax.nn.silu(c)[:, None, :]
    sc_c = jax.nn.silu(c_ctx)[None, None, :]
    h = ctx
    for l in range(DEPTH):
        ctx_out = l < DEPTH - 1
        mod_x = jnp.split(sc_x @ w_mod[l] + b_mod[l], 6, axis=-1)
        mod_c = jnp.split(sc_c @ w_mod[l] + b_mod[l], 6, axis=-1)
        ux = modulate(rmsnorm(x, norm_g[l, 0]), mod_x[0], mod_x[1])
        uc = modulate(rmsnorm(h, norm_g[l, 0]), mod_c[0], mod_c[1])
        mx, mc = token_mixers(ux, uc, w_in[l], mlstm_qk_conv[l], mlstm_gate_bias[l], pool_w[l], pool_scale[l],
                              attn_sink[l], sconv_w[l], w_branch[l], w_out[l], rope, ctx_out)
        x = x + mod_x[2] * rmsnorm(mx, norm_g[l, 1])
        fx = conv_ffn(modulate(rmsnorm(x, norm_g[l, 2]), mod_x[3], mod_x[4]), ffn_up[l], ffn_conv[l], ffn_down[l])
        x = x + mod_x[5] * rmsnorm(fx, norm_g[l, 3])
        if ctx_out:
            h = h + mod_c[2] * rmsnorm(mc, norm_g[l, 1])
            fc = conv_ffn(modulate(rmsnorm(h, norm_g[l, 2]), mod_c[3], mod_c[4]), ffn_up[l], ffn_conv[l], ffn_down[l])
            h = h + mod_c[5] * rmsnorm(fc, norm_g[l, 3])
    return x
```

```python
import contextlib
import numpy as np
import concourse.bass as bass
import concourse.mybir as mybir
from concourse.bass_utils import run_bass_kernel_spmd

F32 = mybir.dt.float32
BF16 = mybir.dt.bfloat16
AF = mybir.ActivationFunctionType
OP = mybir.AluOpType
AX = mybir.AxisListType

D = 1024
KC = 8
DFF = 2816
EPS = 1e-6
FM_QK, FM_O, FM_POOL, FM_AQK, FM_AQKR, FM_SCB, FM_SCC, FM_SCX, FM_MG = 0, 1024, 1536, 2048, 2688, 3328, 3840, 4352, 4864
FM_W = 8960
TM_W = 656


def _cols():
    sp = [512, 512, 512, 512, 16, 512, 512, 128, 128, 512, 512, 512, 4096]
    off = np.concatenate([[0], np.cumsum(sp)])
    seg = lambda i: np.arange(off[i], off[i + 1])

    def rot(cols, nh):
        c = cols.reshape(nh, 2, 2, 16)
        return c[:, :, ::-1, :].reshape(-1)

    aq, ak = seg(6), seg(7)
    fm = np.concatenate([seg(0), seg(1), seg(3), seg(5), aq, ak, rot(aq, 8), rot(ak, 2), seg(9), seg(10), seg(11), seg(12)])
    tm = np.concatenate([seg(2), seg(8), seg(4)])
    assert fm.size == FM_W and tm.size == TM_W
    return fm, tm


class Sem:
    def __init__(self, h):
        self.h = h
        self.v = 0


class Buf:
    __slots__ = ("w", "r", "dsem")

    def __init__(self):
        self.w = None
        self.r = {}
        self.dsem = None


class V:
    def __init__(self, ap, buf):
        self.ap = ap
        self.buf = buf

    def __getitem__(self, idx):
        return V(self.ap[idx], self.buf)

    def bc(self, shape):
        return V(self.ap.to_broadcast(shape), self.buf)

    def re(self, pat, **kw):
        return V(self.ap.rearrange(pat, **kw), self.buf)

    def bitcast(self, dt):
        return V(self.ap.bitcast(dt), self.buf)

    def nb(self):
        return V(self.ap, Buf())

    def bl(self, n):
        sh = list(self.ap.shape) + [n]
        return V(self.ap.unsqueeze(len(sh) - 1).to_broadcast(sh), self.buf)

    def bmid(self, n):
        sh = [self.ap.shape[0], n] + list(self.ap.shape[1:])
        return V(self.ap.unsqueeze(1).to_broadcast(sh), self.buf)


class Eng:
    def __init__(self, name, sem):
        self.name = name
        self.sem = sem
        self.known = {}
        self.prog = []


class K:
    def __init__(self, nc, es, arena_words):
        self.nc = nc
        self.es = es
        self.engs = {}
        for name in ("pe", "act", "dve", "pool", "sp"):
            self.engs[name] = Eng(name, Sem(es.enter_context(nc.semaphore("s_" + name))))
        self.free_dsems = [Sem(es.enter_context(nc.semaphore("d%d" % i))) for i in range(48)]
        self.used_dsems = []
        self.dsem_bufs = []
        self.arena = es.enter_context(nc.sbuf_tensor("arena", [128, arena_words], F32))
        self.aw = arena_words
        self.aoff = 0
        self.pers_off = 0
        self.psum = es.enter_context(nc.psum_tensor("psum", [128, 8, 512], F32))
        self.n_instr = 0

    def alloc(self, shape, dt):
        n = int(np.prod(shape[1:]))
        words = (n * (4 if dt == F32 else 2) + 3) // 4
        words = (words + 7) // 8 * 8
        assert self.aoff + words <= self.aw, ("arena overflow", self.aoff, words, self.aw)
        ap = self.arena[0:shape[0], self.aoff:self.aoff + words]
        self.aoff += words
        if dt != F32:
            ap = ap.bitcast(dt)
        ap = ap[:, 0:n]
        if len(shape) == 3:
            ap = ap.rearrange("p (a b) -> p a b", a=shape[1])
        elif len(shape) == 4:
            ap = ap.rearrange("p (a b c) -> p a b c", a=shape[1], b=shape[2])
        return V(ap, Buf())

    def persist(self):
        self.pers_off = self.aoff

    def bank(self, i, dt=F32):
        ap = self.psum[:, i, :]
        if dt != F32:
            ap = ap.bitcast(dt)
        return V(ap, Buf())

    def _issue(self, en, fn, reads, writes, signal=True, dma_buf=None):
        e = self.engs[en]
        deps = {}

        def add(tok):
            if tok is None:
                return
            s, v = tok
            if deps.get(s, 0) < v:
                deps[s] = v

        for b in reads:
            add(b.w)
        for b in writes:
            add(b.w)
            for s, v in b.r.items():
                add((s, v))
        waits = []
        for s, v in deps.items():
            if en == "pe" and s is e.sem:
                continue
            if e.known.get(s, 0) < v:
                e.known[s] = v
                waits.append((s, v))
        if dma_buf is not None:
            if dma_buf.dsem is None:
                dma_buf.dsem = self.free_dsems.pop()
                self.used_dsems.append(dma_buf.dsem)
                self.dsem_bufs.append(dma_buf)
            s = dma_buf.dsem
            s.v += 16
            tok = (s, s.v)
            inc = (s, 16)
        elif signal:
            e.sem.v += 1
            tok = (e.sem, e.sem.v)
            inc = (e.sem, 1)
        else:
            tok = (e.sem, e.sem.v + 1)
            inc = None
        e.prog.append((waits, fn, inc))
        self.n_instr += 1
        for b in writes:
            b.w = tok
            b.r = {}
        for b in reads:
            if b.w is tok:
                continue
            s, v = tok
            if b.r.get(s, 0) < v:
                b.r[s] = v

    def barrier(self):
        toks = [(e.sem, e.sem.v) for e in self.engs.values() if e.sem.v > 0]
        toks += [(s, s.v) for s in self.used_dsems if s.v > 0]
        for e in self.engs.values():
            waits = []
            for s, v in toks:
                if s is e.sem:
                    continue
                if e.known.get(s, 0) < v:
                    e.known[s] = v
                    waits.append((s, v))
            if waits:
                e.prog.append((waits, None, None))
        self.free_dsems += self.used_dsems
        self.used_dsems = []
        for b in self.dsem_bufs:
            b.dsem = None
        self.dsem_bufs = []
        self.aoff = self.pers_off

    def emit(self):
        nc = self.nc
        with nc.Block() as block:
            def replay(eobj, name):
                for waits, fn, inc in self.engs[name].prog:
                    for s, v in waits:
                        eobj.wait_ge(s.h, v)
                    if fn is not None:
                        ins = fn(eobj)
                        if inc is not None:
                            ins.then_inc(inc[0].h, inc[1])

            @block.tensor
            def _(e):
                replay(e, "pe")

            @block.scalar
            def _(e):
                replay(e, "act")

            @block.vector
            def _(e):
                replay(e, "dve")

            @block.gpsimd
            def _(e):
                replay(e, "pool")

            @block.sync
            def _(e):
                replay(e, "sp")

    @staticmethod
    def _sv(x, reads):
        if isinstance(x, V):
            reads.append(x.buf)
            return x.ap
        return x

    def mm(self, out, lhsT, rhs, start=True, stop=True, signal=None):
        if signal is None:
            signal = stop
        o, l, r = out.ap, lhsT.ap, rhs.ap
        self._issue("pe", lambda e: e.matmul(o, lhsT=l, rhs=r, start=start, stop=stop), [lhsT.buf, rhs.buf], [out.buf], signal)

    def tr(self, out, in_, ident):
        o, i, d = out.ap, in_.ap, ident.ap
        self._issue("pe", lambda e: e.transpose(o, i, d), [in_.buf, ident.buf], [out.buf], True)

    def act(self, out, in_, func, scale=None, bias=None, en="act", extra=()):
        reads = [in_.buf] + [x.buf for x in extra]
        kw = {}
        if scale is not None:
            kw["scale"] = self._sv(scale, reads)
        if bias is not None:
            kw["bias"] = self._sv(bias, reads)
        o, i = out.ap, in_.ap
        self._issue(en, lambda e: e.activation(out=o, in_=i, func=func, **kw), reads, [out.buf])

    def tt(self, en, out, in0, in1, op, extra=()):
        o, a, b = out.ap, in0.ap, in1.ap
        self._issue(en, lambda e: e.tensor_tensor(out=o, in0=a, in1=b, op=op), [in0.buf, in1.buf] + [x.buf for x in extra], [out.buf])

    def ts(self, en, out, in0, s1, op0, s2=None, op1=None):
        reads = [in0.buf]
        a1 = self._sv(s1, reads)
        a2 = self._sv(s2, reads) if s2 is not None else None
        o, a = out.ap, in0.ap
        if op1 is None:
            self._issue(en, lambda e: e.tensor_scalar(out=o, in0=a, scalar1=a1, scalar2=None, op0=op0), reads, [out.buf])
        else:
            self._issue(en, lambda e: e.tensor_scalar(out=o, in0=a, scalar1=a1, scalar2=a2, op0=op0, op1=op1), reads, [out.buf])

    def stt(self, out, in0, scalar, in1, op0, op1):
        reads = [in0.buf, in1.buf]
        s = self._sv(scalar, reads)
        o, a, b = out.ap, in0.ap, in1.ap
        self._issue("dve", lambda e: e.scalar_tensor_tensor(out=o, in0=a, scalar=s, in1=b, op0=op0, op1=op1), reads, [out.buf])

    def copy(self, en, out, in_):
        o, i = out.ap, in_.ap
        if en == "act":
            self._issue(en, lambda e: e.activation(out=o, in_=i, func=AF.Copy), [in_.buf], [out.buf])
        else:
            self._issue(en, lambda e: e.tensor_copy(out=o, in_=i), [in_.buf], [out.buf])

    def recip(self, out, in_):
        o, i = out.ap, in_.ap
        self._issue("dve", lambda e: e.reciprocal(out=o, in_=i), [in_.buf], [out.buf])

    def memset(self, en, out, val):
        o = out.ap
        self._issue(en, lambda e: e.memset(o, val), [], [out.buf])

    def reduce(self, out, in_, op):
        o, i = out.ap, in_.ap
        self._issue("dve", lambda e: e.tensor_reduce(out=o, in_=i, op=op, axis=AX.X), [in_.buf], [out.buf])

    def ld(self, dst, src_ap):
        o = dst.ap
        self._issue("sp", lambda e: e.dma_start(out=o, in_=src_ap), [], [dst.buf], dma_buf=dst.buf)

    def st(self, dst_ap, src):
        i = src.ap
        self._issue("sp", lambda e: e.dma_start(out=dst_ap, in_=i), [src.buf], [], dma_buf=src.buf)


def build(S, L, depth, debug=False):
    T = L + S
    nc = bass.Bass("TRN2", target_bir_lowering=False)
    es = contextlib.ExitStack()
    dbg_kind = "ExternalOutput" if debug else "Internal"

    def din(name, shape, dt=F32):
        return nc.dram_tensor(name, list(shape), dt, kind="ExternalInput").ap()

    def dsc(name, shape, dt):
        return nc.dram_tensor(name, list(shape), dt, kind=dbg_kind).ap()

    xin = din("xin", [D, T])
    sc_in = din("sc_in", [128, KC * 2])
    cf32 = din("cf32", [128, 3 * 128])
    cosT = din("cosT", [128, T])
    sinT = din("sinT", [128, T])
    invc = din("invc", [128, 4 * T])
    w_mod = din("w_mod", [depth, D, 6 * D])
    b_mod = din("b_mod", [depth, 128, 48])
    norm_g = din("norm_g", [depth, 128, 32])
    w_fm = din("w_fm", [depth, D, FM_W])
    w_tm = din("w_tm", [depth, D, TM_W])
    qkcw = din("qkcw", [depth, 128, 24])
    gbias = din("gbias", [depth, 128, 16])
    poolw = din("poolw", [depth, 128, 512])
    pscale = din("pscale", [depth, 128, 4])
    sink = din("sink", [depth, 128, 8])
    scw = din("scw", [depth, 128, 12])
    w_br = din("w_br", [depth, 2048, D])
    w_out = din("w_out", [depth, D, D])
    f_up = din("f_up", [depth, D, 2 * DFF])
    f_cw = din("f_cw", [depth, 128, 132])
    f_dn = din("f_dn", [depth, DFF, D])
    yout = nc.dram_tensor("yout", [D, S], F32, kind="ExternalOutput").ap()

    U = dsc("U", [D, T], BF16)
    PXF = dsc("PXF", [FM_W, T], BF16)
    TMV = dsc("TMV", [T, 640], BF16)
    TMG = dsc("TMG", [T, 16], F32)
    QKC = dsc("QKC", [D, T], BF16)
    AQKR = dsc("AQKR", [640, T], BF16)
    YS = dsc("YS", [2048, T], BF16)
    HF = dsc("HF", [T, 512], F32)
    X1 = dsc("X1", [D, T], F32)
    X2 = dsc("X2", [D, T], F32)
    AU = dsc("AU", [2 * DFF, T], BF16)

    k = K(nc, es, 49500)

    def fmv(dr, r0, nchunk, t0, n):
        return dr[r0:r0 + 128 * nchunk, t0:t0 + n].rearrange("(c p) t -> p c t", p=128)

    def tiles(n_lat, with_ctx=True):
        out = []
        if with_ctx:
            out += [(1, t, min(n_lat, L - t)) for t in range(0, L, n_lat)]
        out += [(0, L + t, n_lat) for t in range(0, S, n_lat)]
        return out

    c_f32 = k.alloc([128, 3, 128], F32)
    k.ld(c_f32, cf32.rearrange("p (a b) -> p a b", a=3))
    ident_f, triF, triB = c_f32[:, 0, :], c_f32[:, 1, :], c_f32[:, 2, :]
    c_bf = k.alloc([128, 3, 128], BF16)
    k.copy("dve", c_bf, c_f32)
    ident_b, maskN, maskP = c_bf[:, 0, :], c_bf[:, 1, :], c_bf[:, 2, :]
    onesm = k.alloc([128, 128], BF16)
    k.memset("dve", onesm, 1.0 / D)
    ones_b = k.alloc([128, 128], BF16)
    k.memset("dve", ones_b, 1.0)
    ones_f = k.alloc([128, 128], F32)
    k.memset("dve", ones_f, 1.0)
    VEC = k.alloc([128, depth * 6, KC, 2], F32)
    sc_t = k.alloc([128, KC, 2], F32)
    k.ld(sc_t, sc_in.rearrange("p (a b) -> p a b", a=KC))
    sc_s = k.alloc([128, KC, 2], F32)
    k.act(sc_s, sc_t, AF.Silu)
    p_qkc = k.alloc([128, 8, 3], F32)
    p_gb = k.alloc([128, 16], F32)
    p_pw = k.alloc([128, 4, 128], BF16)
    p_ps = k.alloc([128, 4], F32)
    p_es = k.alloc([128, 8], F32)
    p_scw = k.alloc([128, 4, 3], F32)
    p_fcw = k.alloc([128, 44, 3], F32)
    k.persist()

    for l in range(depth):
        mod = k.alloc([128, 48, 2], F32)
        bm = k.alloc([128, 48], F32)
        k.ld(bm, b_mod[l])
        ng = k.alloc([128, 4, KC], F32)
        k.ld(ng, norm_g[l].rearrange("p (a b) -> p a b", a=4))
        wst = [k.alloc([128, KC, 768], F32) for _ in range(2)]
        for cg in range(8):
            w = wst[cg % 2]
            k.ld(w, w_mod[l][:, cg * 768:(cg + 1) * 768].rearrange("(c p) n -> p c n", p=128))
            ps = k.bank(cg % 2)
            for fc in range(6):
                for kc in range(KC):
                    k.mm(ps[:, fc * 2:fc * 2 + 2], w[:, kc, fc * 128:(fc + 1) * 128], sc_s[:, kc, :], start=(kc == 0), stop=(kc == KC - 1))
            k.tt("dve", mod[:, cg * 6:(cg + 1) * 6, :], ps[:, 0:12].re("p (a b) -> p a b", a=6),
                 bm[:, cg * 6:(cg + 1) * 6].bl(2), OP.add)
        mv = lambda j: mod[:, j * 8:(j + 1) * 8, :]
        gv = lambda j: ng[:, j, :].bl(2)
        vv = lambda j: VEC[:, l * 6 + j, :, :]
        tmp = k.alloc([128, KC, 2], F32)
        k.ts("dve", tmp, mv(1), 1.0, OP.add)
        k.tt("dve", vv(0), tmp, gv(0), OP.mult)
        k.copy("dve", vv(1), mv(0))
        k.tt("dve", vv(2), mv(2), gv(1), OP.mult)
        tmp2 = k.alloc([128, KC, 2], F32)
        k.ts("dve", tmp2, mv(4), 1.0, OP.add)
        k.tt("dve", vv(3), tmp2, gv(2), OP.mult)
        k.copy("dve", vv(4), mv(3))
        k.tt("dve", vv(5), mv(5), gv(3), OP.mult)
        k.barrier()

    def norm_tmp(nmax):
        return (k.alloc([128, KC, nmax], BF16), k.alloc([128, nmax], F32), k.alloc([128, KC, nmax], F32))

    def rms_scale(f, n, ps, tmp):
        sq, rs = tmp[0][:, :, 0:n], tmp[1][:, 0:n]
        k.act(sq, f, AF.Square)
        for c in range(KC):
            k.mm(ps[:, 0:n], onesm, sq[:, c, :], start=(c == 0), stop=(c == KC - 1))
        k.act(rs, ps[:, 0:n], AF.Sqrt, bias=epsv)
        k.recip(rs, rs)
        return rs

    def norm_mod(x, n, l, which, s, ps, out_u, tmp):
        rs = rms_scale(x, n, ps, tmp)
        t = tmp[2][:, :, 0:n]
        k.tt("dve", t, x, rs.bmid(KC), OP.mult)
        for c in range(KC):
            k.act(out_u[:, c, :], t[:, c, :], AF.Identity, scale=VEC[:, l * 6 + which, c, s:s + 1], bias=VEC[:, l * 6 + which + 1, c, s:s + 1])

    epsv = k.alloc([128, 1], F32)
    k.memset("dve", epsv, EPS)
    k.persist()

    def cast_w(i, dst, src):
        en = ("dve", "pool", "act")[i % 3]
        k.copy(en, dst, src)

    def proj_fm(l, wsrc, ncols, dst, tls_all, has_tm, sig_from=None):
        groups = [(g0, min(512, ncols - g0)) for g0 in range(0, ncols, 512)]
        sup, cur, cnt = [], [], 0
        for tl in tls_all:
            if cnt + tl[2] > 4352:
                sup.append(cur)
                cur, cnt = [], 0
            cur.append(tl)
            cnt += tl[2]
        sup.append(cur)
        for tls in sup:
            base = tls[0][1]
            ntok = sum(t[2] for t in tls)
            ures = k.alloc([128, KC, ntok], BF16)
            for (s, t0, n) in tls:
                k.ld(ures[:, :, t0 - base:t0 - base + n], fmv(U, 0, KC, t0, n))
            wst = [k.alloc([128, KC, 512], F32) for _ in range(2)]
            wbf = [k.alloc([128, KC, 512], BF16) for _ in range(2)]
            stg = [k.alloc([128, 4, 512], BF16) for _ in range(3)]
            banks = [k.bank(i) for i in range(8)]
            bi, si = 0, 0
            if has_tm:
                wtf = k.alloc([128, KC, TM_W], F32)
                k.ld(wtf, w_tm[l].rearrange("(c p) n -> p c n", p=128))
                wtb = k.alloc([128, KC, TM_W], BF16)
                k.copy("pool", wtb, wtf)
                tst = [k.alloc([128, 640], BF16) for _ in range(2)]
                gst = [k.alloc([128, 16], F32) for _ in range(2)]
                ti = 0
                for (s, t0, n) in tls:
                    for b0 in range(0, n, 128):
                        pa, pb = banks[bi % 8], banks[(bi + 1) % 8]
                        bi += 2
                        lt = ures[:, :, t0 - base + b0:t0 - base + b0 + 128]
                        for kc in range(KC):
                            k.mm(pa, lt[:, kc, :], wtb[:, kc, 0:512], start=(kc == 0), stop=(kc == KC - 1))
                        for kc in range(KC):
                            k.mm(pb[:, 0:144], lt[:, kc, :], wtb[:, kc, 512:656], start=(kc == 0), stop=(kc == KC - 1))
                        ts_, gs_ = tst[ti % 2], gst[ti % 2]
                        ti += 1
                        k.copy("act", ts_[:, 0:512], pa)
                        k.copy("dve", ts_[:, 512:640], pb[:, 0:128])
                        k.copy("dve", gs_, pb[:, 128:144])
                        k.st(TMV[t0 + b0:t0 + b0 + 128, :], ts_)
                        k.st(TMG[t0 + b0:t0 + b0 + 128, :], gs_)
            def prefetch(gi_):
                g0_, gw_ = groups[gi_]
                k.ld(wst[gi_ % 2][:, :, 0:gw_], wsrc[:, g0_:g0_ + gw_].rearrange("(c p) n -> p c n", p=128))
                k.copy("pool", wbf[gi_ % 2][:, :, 0:gw_], wst[gi_ % 2][:, :, 0:gw_])

            prefetch(0)
            for gi, (g0, gw) in enumerate(groups):
                if gi + 1 < len(groups):
                    prefetch(gi + 1)
                wb = wbf[gi % 2]
                nmc = gw // 128
                for (s, t0, n) in tls:
                    sg = stg[si % 3]
                    si += 1
                    for mc in range(nmc):
                        ps = banks[bi % 8]
                        bi += 1
                        for kc in range(KC):
                            k.mm(ps[:, 0:n], wb[:, kc, mc * 128:(mc + 1) * 128], ures[:, kc, t0 - base:t0 - base + n], start=(kc == 0), stop=(kc == KC - 1))
                        if sig_from is not None and g0 + mc * 128 >= sig_from:
                            k.act(sg[:, mc, 0:n], ps[:, 0:n], AF.Sigmoid)
                        else:
                            k.copy("act" if mc % 2 == 0 else "dve", sg[:, mc, 0:n], ps[:, 0:n])
                    k.st(fmv(dst, g0, nmc, t0, n), sg[:, 0:nmc, 0:n])
            k.barrier()

    Xcur = xin
    for l in range(depth):
        last = (l == depth - 1)
        k.ld(p_qkc, qkcw[l].rearrange("p (a b) -> p a b", a=8))
        k.ld(p_gb, gbias[l])
        pwf = k.alloc([128, 4, 128], F32)
        k.ld(pwf, poolw[l].rearrange("p (a b) -> p a b", a=4))
        k.copy("dve", p_pw, pwf)
        k.ld(p_ps, pscale[l])
        skt = k.alloc([128, 8], F32)
        k.ld(skt, sink[l])
        k.act(p_es, skt, AF.Exp)
        k.ld(p_scw, scw[l].rearrange("p (a b) -> p a b", a=4))
        k.ld(p_fcw, f_cw[l].rearrange("p (a b) -> p a b", a=44))
        k.barrier()

        if l == 0:
            xs = [k.alloc([128, KC, 512], F32) for _ in range(2)]
            us = [k.alloc([128, KC, 512], BF16) for _ in range(2)]
            tmps = [norm_tmp(512) for _ in range(2)]
            pbs = [k.bank(0), k.bank(1)]
            for i, (s, t0, n) in enumerate(tiles(512)):
                x, u = xs[i % 2], us[i % 2]
                k.ld(x[:, :, 0:n], fmv(Xcur, 0, KC, t0, n))
                norm_mod(x[:, :, 0:n], n, l, 0, s, pbs[i % 2], u[:, :, 0:n], tmps[i % 2])
                k.st(fmv(U, 0, KC, t0, n), u[:, :, 0:n])
            k.barrier()

        proj_fm(l, w_fm[l], FM_W, PXF, tiles(512), True, sig_from=FM_MG)
        if debug == "A":
            break

        def halo_ld(dst, dr, r0, nch, s, t0, n, h):
            lo, hi = (0, L) if s == 1 else (L, T)
            a, b = max(t0 - h, lo), min(t0 + n + h, hi)
            o = t0 - h
            if a > o:
                k.memset("pool", dst[:, :, 0:a - o], 0.0)
            if b < t0 + n + h:
                k.memset("pool", dst[:, :, b - o:n + 2 * h], 0.0)
            k.ld(dst[:, :, a - o:b - o], fmv(dr, r0, nch, a, b - a))

        def conv3(acc, src, w, c, n, first="act"):
            k.act(acc, src[:, c, 0:n], AF.Copy, scale=w[:, c, 0:1])
            k.stt(acc, src[:, c, 1:n + 1], w[:, c, 1:2], acc, OP.mult, OP.add)
            k.stt(acc, src[:, c, 2:n + 2], w[:, c, 2:3], acc, OP.mult, OP.add)

        NB = 2
        qin = [k.alloc([128, 8, 514], BF16) for _ in range(NB)]
        qacc_all = k.alloc([128, 8, 512], F32)
        qaccs = [qacc_all[:, c, :].nb() for c in range(8)]
        qo = [k.alloc([128, 8, 512], BF16) for _ in range(NB)]
        sB = [k.alloc([128, 4, 512], BF16) for _ in range(NB)]
        sCX = [k.alloc([128, 8, 514], BF16) for _ in range(NB)]
        sprod = [k.alloc([128, 4, 514], F32) for _ in range(1)] * 2
        sacc_all = k.alloc([128, 4, 512], F32)
        saccs = [sacc_all[:, c, :].nb() for c in range(4)]
        so = [k.alloc([128, 4, 512], BF16) for _ in range(NB)]
        pin = [k.alloc([128, 4, 528], BF16) for _ in range(NB)]
        pwa = k.alloc([128, 4, 528], F32)
        pwb = k.alloc([128, 4, 528], F32)
        pic = [k.alloc([128, 4, 512], F32) for _ in range(1)] * 2
        pdf = [k.alloc([128, 512], BF16) for _ in range(4)]
        po = [k.alloc([128, 4, 512], BF16) for _ in range(NB)]
        rx = [k.alloc([128, 10, 512], BF16) for _ in range(1)] * 2
        rcs = [k.alloc([128, 2, 512], F32) for _ in range(1)] * 2
        rt = [k.alloc([128, 2, 5, 512], F32) for _ in range(1)]
        ro = [k.alloc([128, 5, 512], BF16) for _ in range(1)] * 2
        pbk = [k.bank(i) for i in range(4)]
        for i, (s, t0, n) in enumerate(tiles(512)):
            j = i % NB
            halo_ld(qin[j][:, :, 0:n + 2], PXF, FM_QK, 8, s, t0, n, 1)
            for c in range(8):
                k.act(qaccs[c][:, 0:n], qin[j][:, c, 0:n], AF.Copy, scale=p_qkc[:, c, 0:1])
            for tap in (1, 2):
                for c in range(8):
                    k.stt(qaccs[c][:, 0:n], qin[j][:, c, tap:n + tap], p_qkc[:, c, tap:tap + 1], qaccs[c][:, 0:n], OP.mult, OP.add)
            k.act(qo[j][:, :, 0:n], qacc_all[:, :, 0:n], AF.Silu, extra=qaccs)
            k.st(fmv(QKC, 0, 8, t0, n), qo[j][:, :, 0:n])
            k.ld(sB[j][:, :, 0:n], fmv(PXF, FM_SCB, 4, t0, n))
            halo_ld(sCX[j][:, :, 0:n + 2], PXF, FM_SCC, 8, s, t0, n, 1)
            k.tt("pool", sprod[j][:, :, 0:n + 2], sCX[j][:, 0:4, 0:n + 2], sCX[j][:, 4:8, 0:n + 2], OP.mult)
            for c in range(4):
                k.act(saccs[c][:, 0:n], sprod[j][:, c, 0:n], AF.Copy, scale=p_scw[:, c, 0:1])
            for tap in (1, 2):
                for c in range(4):
                    k.stt(saccs[c][:, 0:n], sprod[j][:, c, tap:n + tap], p_scw[:, c, tap:tap + 1], saccs[c][:, 0:n], OP.mult, OP.add)
            k.tt("pool", so[j][:, :, 0:n], sacc_all[:, :, 0:n], sB[j][:, :, 0:n], OP.mult, extra=saccs)
            k.st(fmv(YS, 1536, 4, t0, n), so[j][:, :, 0:n])
            halo_ld(pin[j][:, :, 0:n + 16], PXF, FM_POOL, 4, s, t0, n, 8)
            k.ld(pic[j][:, :, 0:n], invc.rearrange("p (g t) -> p g t", g=4)[:, :, t0:t0 + n])
            P_ = pin[j]
            k.tt("pool", pwa[:, :, 1:n + 16], P_[:, :, 0:n + 15], P_[:, :, 1:n + 16], OP.add)
            k.tt("pool", pwb[:, 1:4, 2:n + 15], pwa[:, 1:4, 1:n + 14], pwa[:, 1:4, 3:n + 16], OP.add)
            k.tt("pool", pwa[:, 2:4, 4:n + 13], pwb[:, 2:4, 2:n + 11], pwb[:, 2:4, 6:n + 15], OP.add)
            k.tt("pool", pwb[:, 3:4, 8:n + 8], pwa[:, 3:4, 4:n + 4], pwa[:, 3:4, 12:n + 12], OP.add)
            curs = [pwa[:, 0, 8:n + 8], pwb[:, 1, 8:n + 8], pwa[:, 2, 8:n + 8], pwb[:, 3, 8:n + 8]]
            for g in range(4):
                k.tt("dve", curs[g], curs[g], pic[j][:, g, 0:n], OP.mult)
            for g in range(4):
                k.tt("dve", pdf[g][:, 0:n], curs[g], P_[:, g, 8:n + 8], OP.subtract)
            for g in range(4):
                k.mm(pbk[g][:, 0:n], p_pw[:, g, :], pdf[g][:, 0:n])
            for g in range(4):
                k.act(po[j][:, g, 0:n], pbk[g][:, 0:n], AF.Copy, scale=p_ps[:, g:g + 1])
            k.st(fmv(YS, 512, 4, t0, n), po[j][:, :, 0:n])
            k.ld(rx[j][:, :, 0:n], fmv(PXF, FM_AQK, 10, t0, n))
            k.ld(rcs[j][:, 0, 0:n], cosT[:, t0:t0 + n])
            k.ld(rcs[j][:, 1, 0:n], sinT[:, t0:t0 + n])
            k.tt("dve", rt[0][:, 0, :, 0:n], rx[j][:, 0:5, 0:n], rcs[j][:, 0, 0:n].bmid(5), OP.mult)
            k.tt("pool", rt[0][:, 1, :, 0:n], rx[j][:, 5:10, 0:n], rcs[j][:, 1, 0:n].bmid(5), OP.mult)
            k.tt("dve", ro[j][:, :, 0:n], rt[0][:, 0, :, 0:n], rt[0][:, 1, :, 0:n], OP.add)
            k.st(fmv(AQKR, 0, 5, t0, n), ro[j][:, :, 0:n])
        k.barrier()
        if debug == "B0":
            break

        qT = [k.alloc([64, 4, 512], BF16) for _ in range(2)]
        kT = [k.alloc([64, 768], BF16) for _ in range(2)]
        vT = [k.alloc([128, 6, 64], BF16) for _ in range(2)]
        cK = [k.alloc([64, L], BF16) for _ in range(2)]
        cV = [k.alloc([128, L // 128, 64], BF16) for _ in range(2)]
        Eb = [k.alloc([128, 4, 128], BF16) for _ in range(4)]
        den = [k.alloc([64, 4, 128], F32) for _ in range(2)]
        ost = [k.alloc([64, 4, 512], BF16) for _ in range(2)]
        psS = [k.bank(0), k.bank(1), k.bank(2)]
        psO = [k.bank(3), k.bank(4)]
        psD = [k.bank(5), k.bank(6)]
        r4 = lambda v: v.re("p (r q) -> p r q", r=4)
        cnt = ecnt = ocnt = ti = 0
        for g in range(2):
            k.ld(cK[g], AQKR[512 + g * 64:512 + (g + 1) * 64, 0:L])
            k.ld(cV[g], TMV[0:L, 512 + g * 64:512 + (g + 1) * 64].rearrange("(b p) d -> p b d", p=128))
        for (s, t0, n) in tiles(512, with_ctx=not last):
            for g in range(2):
                j = ti % 2
                ti += 1
                k.ld(qT[j][:, :, 0:n], AQKR[g * 256:(g + 1) * 256, t0:t0 + n].rearrange("(r d) t -> d r t", d=64))
                o = t0 - 128
                if s == 0:
                    a, b = max(o, L), min(t0 + n + 128, T)
                    k.ld(kT[j][:, a - o:b - o], AQKR[512 + g * 64:512 + (g + 1) * 64, a:b])
                    k.ld(vT[j][:, (a - o) // 128:(b - o) // 128, :], TMV[a:b, 512 + g * 64:512 + (g + 1) * 64].rearrange("(b p) d -> p b d", p=128))
                for qb in range(n // 128):
                    tq = t0 + qb * 128
                    blocks = [(cK[g][:, c * 128:(c + 1) * 128], cV[g][:, c, :], None) for c in range(L // 128)]
                    if s == 0:
                        for off, msk in ((-128, maskP), (0, None), (128, maskN)):
                            tk = tq + off
                            if L <= tk < T:
                                blocks.append((kT[j][:, tk - o:tk - o + 128], vT[j][:, (tk - o) // 128, :], msk))
                    pO, pD = psO[ocnt % 2], psD[ocnt % 2]
                    ocnt += 1
                    for bi_, (kk, vv, msk) in enumerate(blocks):
                        ps = psS[cnt % 3]
                        cnt += 1
                        k.mm(r4(ps), kk, qT[j][:, :, qb * 128:(qb + 1) * 128])
                        e = Eb[ecnt % 4]
                        ecnt += 1
                        k.act(e, r4(ps), AF.Exp, scale=0.125)
                        if msk is not None:
                            k.tt("pool", e, e, msk.bmid(4), OP.mult)
                        k.mm(r4(pO[0:64, :]), vv, e, start=(bi_ == 0), stop=(bi_ == len(blocks) - 1))
                        k.mm(r4(pD[0:64, :]), ones_b[:, 0:64], e, start=(bi_ == 0), stop=(bi_ == len(blocks) - 1))
                    dn = den[ocnt % 2]
                    k.tt("dve", dn, r4(pD[0:64, :]), p_es[0:64, g * 4:(g + 1) * 4].bl(128), OP.add)
                    k.recip(dn, dn)
                    k.tt("dve", ost[j][:, :, qb * 128:(qb + 1) * 128], r4(pO[0:64, :]), dn, OP.mult)
                k.st(YS[1024 + g * 256:1024 + (g + 1) * 256, t0:t0 + n].rearrange("(r d) t -> d r t", d=64), ost[j][:, :, 0:n])

        G = [k.alloc([128, 129], F32) for _ in range(4)]
        Cb = [k.alloc([128, 129], BF16) for _ in range(4)]
        eLs = [k.alloc([128, 4], F32) for _ in range(2)]
        lnsc = k.alloc([128, 1], F32)
        k.memset("dve", lnsc, float(np.log(128.0 ** -0.5)))
        one1 = k.alloc([128, 1], F32)
        k.memset("dve", one1, 1.0)
        qk_t = [k.alloc([128, 8, 512], BF16) for _ in range(2)]
        Vp = [k.alloc([128, 4, 4, 129], BF16) for _ in range(2)]
        Gt = [k.alloc([128, 4, 16], F32) for _ in range(2)]
        HFt = [k.alloc([128, 4, 512], F32) for _ in range(2)]
        ot = [k.alloc([128, 4, 512], BF16) for _ in range(2)]
        sgo = [k.alloc([128, 4, 512], BF16) for _ in range(2)]
        YAs = [k.alloc([128, 4, 512], BF16) for _ in range(2)]
        gsm = [[k.alloc([128, 16], F32), k.alloc([128, 4], F32), k.alloc([128, 4], F32), k.alloc([128, 4], F32),
                k.alloc([128, 4], F32), k.alloc([128, 4], F32)] for _ in range(2)]
        ST = [[k.alloc([128, 128], BF16) for _ in range(4)] for _ in range(2)]
        kt = [[k.alloc([128, 128], BF16) for _ in range(4)] for _ in range(2)]
        md = [[k.alloc([128, 1], F32) for _ in range(4)] for _ in range(2)]
        hs = [[k.alloc([128, 128], F32) for _ in range(4)] for _ in range(2)]
        for v_ in Vp:
            k.memset("pool", v_[:, :, :, 128:129], 1.0)
        b0 = k.bank(0)
        pS = [b0[:, h * 128:(h + 1) * 128].nb() for h in range(4)]
        b1 = k.bank(1, BF16)
        pK = [b1[:, h * 128:(h + 1) * 128].nb() for h in range(4)]
        b23 = [k.bank(2), k.bank(3)]
        pH = [b23[h // 2][:, (h % 2) * 256:(h % 2) * 256 + 129].nb() for h in range(4)]
        b45 = [k.bank(4), k.bank(5)]
        pC = [b45[h // 2][:, (h % 2) * 256:(h % 2) * 256 + 129].nb() for h in range(4)]
        pG = [k.bank(6)[:, 0:8].nb(), k.bank(6)[:, 8:16].nb()]
        b7 = k.bank(7)
        pT = [b7[:, h * 128:(h + 1) * 128].nb() for h in range(4)]
        bcnt = 0
        for d in range(2):
            for h in range(4):
                k.memset("dve", G[h], 0.0)
                k.memset("pool", Cb[h], 0.0)
            k.memset("dve", eLs[0], 1.0)
            k.memset("dve", eLs[1], 1.0)
            tl = tiles(512)
            ctx_t = [t for t in tl if t[0] == 1]
            lat_t = [t for t in tl if t[0] == 0]
            order = (ctx_t + lat_t) if d == 0 else (ctx_t[::-1] + lat_t[::-1])
            tri_d = triF if d == 0 else triB
            msk_d = maskN if d == 0 else maskP
            for ti_, (s, t0, n) in enumerate(order):
                j = ti_ % 2
                nbk = n // 128
                outp = not (last and s == 1)
                k.ld(qk_t[j][:, :, 0:n], fmv(QKC, 0, 8, t0, n))
                for b in range(nbk):
                    k.ld(Vp[j][:, b, :, 0:128], TMV[t0 + b * 128:t0 + (b + 1) * 128, 0:512].rearrange("p (h e) -> p h e", h=4))
                k.ld(Gt[j][:, 0:nbk, :], TMG[t0:t0 + n, :].rearrange("(b p) c -> p b c", p=128))
                if d == 1 and outp:
                    k.ld(HFt[j][:, 0:nbk, :], HF[t0:t0 + n, :].rearrange("(b p) c -> p b c", p=128))
                    k.ld(ot[j][:, :, 0:n], fmv(PXF, FM_O, 4, t0, n))
                    k.act(sgo[j][:, :, 0:n], ot[j][:, :, 0:n], AF.Sigmoid)
                for b in (range(nbk) if d == 0 else range(nbk - 1, -1, -1)):
                    jj = bcnt % 2
                    bcnt += 1
                    gp, e1, l1, tmp_, sc, ebn = gsm[jj]
                    eLc, eLp = eLs[jj], eLs[1 - jj]
                    k.tt("dve", gp, Gt[j][:, b, :], p_gb, OP.add)
                    k.act(e1, gp[:, 8 + 4 * d:12 + 4 * d], AF.Exp, scale=-1.0)
                    k.act(l1, e1, AF.Ln, bias=one1)
                    pg = pG[jj]
                    k.mm(pg[:, 0:4], tri_d, l1)
                    k.mm(pg[:, 4:8], ones_f, l1)
                    k.tt("dve", tmp_, gp[:, 4 * d:4 * d + 4], pg[:, 0:4], OP.add)
                    k.act(sc, tmp_, AF.Exp, bias=lnsc)
                    k.act(ebn, pg[:, 0:4], AF.Exp)
                    k.act(eLc, pg[:, 4:8], AF.Exp, scale=-1.0)
                    bs = slice(b * 128, (b + 1) * 128)
                    for h in range(4):
                        qT_ = qk_t[j][:, h, bs]
                        kT_ = qk_t[j][:, 4 + h, bs]
                        vp = Vp[j][:, b, h, :]
                        k.mm(pS[h], kT_, qT_)
                        k.stt(ST[jj][h], pS[h], sc[:, h:h + 1], msk_d, OP.mult, OP.mult)
                        k.tr(pK[h], kT_, ident_b)
                        k.act(kt[jj][h], pK[h], AF.Copy, scale=sc[:, h:h + 1])
                        k.mm(pH[h], ST[jj][h], vp, start=True, stop=False)
                        k.mm(pH[h], qT_, Cb[h], start=False, stop=True)
                        k.mm(pC[h], kt[jj][h], vp)
                        m_ = md[jj][h]
                        k.act(m_, pH[h][:, 128:129], AF.Abs)
                        k.tt("dve", m_, m_, ebn[:, h:h + 1], OP.max)
                        k.recip(m_, m_)
                        if d == 0:
                            k.act(HFt[j][:, b, h * 128:(h + 1) * 128], pH[h][:, 0:128], AF.Copy, scale=m_)
                        elif outp:
                            k.stt(hs[jj][h], pH[h][:, 0:128], m_, HFt[j][:, b, h * 128:(h + 1) * 128], OP.mult, OP.add)
                            k.tr(pT[h], hs[jj][h], ident_f)
                            k.tt("dve", YAs[j][:, h, bs], pT[h], sgo[j][:, h, bs], OP.mult)
                        k.stt(G[h], G[h], eLp[:, h:h + 1], pC[h], OP.mult, OP.add)
                        k.act(Cb[h], G[h], AF.Copy, scale=eLc[:, h:h + 1])
                if d == 0:
                    k.st(HF[t0:t0 + n, :].rearrange("(b p) c -> p b c", p=128), HFt[j][:, 0:nbk, :])
                elif outp:
                    k.st(fmv(YS, 0, 4, t0, n), YAs[j][:, :, 0:n])
            k.barrier() if d == 0 else None
            if d == 0:
                pass
        k.barrier()
        if debug == "B":
            break

        def resident_weights(specs):
            save = k.pers_off
            dsts = [k.alloc([128, nk, 1024], BF16) for nk, _ in specs]
            k.pers_off = k.aoff
            wst = [k.alloc([128, 2, 1024], F32) for _ in range(2)]
            i = 0
            for dst, (nk, src) in zip(dsts, specs):
                for pc_ in range(nk // 2):
                    k.ld(wst[i % 2], src[pc_ * 256:(pc_ + 1) * 256, :].rearrange("(c p) d -> p c d", p=128))
                    cast_w(i, dst[:, pc_ * 2:(pc_ + 1) * 2, :], wst[i % 2])
                    i += 1
            k.barrier()
            return dsts, save

        (wbr, wo), pers_save = resident_weights([(16, w_br[l]), (8, w_out[l])])
        NC_ = 256
        ys = [k.alloc([128, 16, NC_], BF16) for _ in range(2)]
        mg = [k.alloc([128, 32, NC_], BF16) for _ in range(2)]
        xt = [k.alloc([128, 8, NC_], F32) for _ in range(2)]
        tprod = [k.alloc([128, 4, NC_], F32) for _ in range(2)]
        z = k.alloc([128, 8, NC_], BF16)
        zf = k.alloc([128, 8, NC_], F32)
        mxs = k.alloc([128, 8, NC_], F32)
        ntmp = norm_tmp(NC_)
        xm = [k.alloc([128, 8, NC_], F32) for _ in range(2)]
        u2 = [k.alloc([128, 8, NC_], BF16) for _ in range(2)]
        ps4 = [V(k.psum[:, 2 * i:2 * i + 2, :].rearrange("p b (a n) -> p (b a) n", a=2), Buf()) for i in range(2)]
        psm = [k.bank(4), k.bank(5)]
        psr = [k.bank(6), k.bank(7)]
        for i, (s, t0, n) in enumerate(tiles(NC_, with_ctx=not last)):
            j = i % 2
            k.ld(ys[j][:, :, 0:n], fmv(YS, 0, 16, t0, n))
            k.ld(mg[j][:, :, 0:n], fmv(PXF, FM_MG, 32, t0, n))
            k.ld(xt[j][:, :, 0:n], fmv(Xcur, 0, 8, t0, n))
            for dc in range(8):
                p4 = ps4[dc % 2]
                for nb_ in range(4):
                    for kc in range(4):
                        k.mm(p4[:, nb_, 0:n], wbr[:, nb_ * 4 + kc, dc * 128:(dc + 1) * 128], ys[j][:, nb_ * 4 + kc, 0:n], start=(kc == 0), stop=(kc == 3))
                tp = tprod[dc % 2]
                k.tt("dve", tp[:, :, 0:n], p4[:, :, 0:n], mg[j][:, dc::8, 0:n], OP.mult)
                if dc >= 1:
                    k.reduce(zf[:, dc - 1, 0:n], tprod[(dc - 1) % 2][:, :, 0:n].re("p a n -> p n a"), OP.add)
            k.reduce(zf[:, 7, 0:n], tprod[1][:, :, 0:n].re("p a n -> p n a"), OP.add)
            k.copy("pool", z[:, :, 0:n], zf[:, :, 0:n])
            for dc in range(8):
                ps = psm[dc % 2]
                for kc in range(8):
                    k.mm(ps[:, 0:n], wo[:, kc, dc * 128:(dc + 1) * 128], z[:, kc, 0:n], start=(kc == 0), stop=(kc == 7))
                k.copy("act", mxs[:, dc, 0:n], ps[:, 0:n])
            rs = rms_scale(mxs[:, :, 0:n], n, psr[0], ntmp)
            t_ = ntmp[2][:, :, 0:n]
            k.tt("dve", t_, mxs[:, :, 0:n], rs.bmid(KC), OP.mult)
            for c in range(KC):
                k.stt(xm[j][:, c, 0:n], t_[:, c, :], VEC[:, l * 6 + 2, c, s:s + 1], xt[j][:, c, 0:n], OP.mult, OP.add)
            k.st(fmv(X1, 0, 8, t0, n), xm[j][:, :, 0:n])
            norm_mod(xm[j][:, :, 0:n], n, l, 3, s, psr[1], u2[j][:, :, 0:n], ntmp)
            k.st(fmv(U, 0, 8, t0, n), u2[j][:, :, 0:n])
        k.pers_off = pers_save
        k.barrier()
        if debug == "C":
            break

        proj_fm(l, f_up[l], 2 * DFF, AU, tiles(512, with_ctx=not last), False)

        (wd,), pers_save = resident_weights([(22, f_dn[l])])
        a_t = [k.alloc([128, 44, NC_ + 2], BF16) for _ in range(2)]
        facc = [k.alloc([128, NC_], F32) for _ in range(22)]
        gg2 = [[k.alloc([128, NC_], BF16) for _ in range(22)] for _ in range(2)]
        xmt = [k.alloc([128, 8, NC_], F32) for _ in range(2)]
        fs = k.alloc([128, 8, NC_], F32)
        ntmp = norm_tmp(NC_)
        xn = k.alloc([128, 8, NC_], F32)
        un = [k.alloc([128, 8, NC_], BF16) for _ in range(1)] * 2
        psm = [k.bank(0), k.bank(1)]
        psr = [k.bank(2), k.bank(3)]
        Xn = yout if last else X2
        for i, (s, t0, n) in enumerate(tiles(NC_, with_ctx=not last)):
            j = i % 2
            halo_ld(a_t[j][:, :, 0:n + 2], AU, 0, 44, s, t0, n, 1)
            k.ld(xmt[j][:, :, 0:n], fmv(X1, 0, 8, t0, n))
            gg = gg2[j]
            at = a_t[j]
            for half in range(2):
                cs = list(range(half * 11, half * 11 + 11))
                for i_, c in enumerate(cs):
                    k.act(facc[i_][:, 0:n], at[:, c, 0:n], AF.Copy, scale=p_fcw[:, c, 0:1])
                    k.act(facc[11 + i_][:, 0:n], at[:, 22 + c, 0:n], AF.Copy, scale=p_fcw[:, 22 + c, 0:1])
                for tap in (1, 2):
                    for i_, c in enumerate(cs):
                        k.stt(facc[i_][:, 0:n], at[:, c, tap:n + tap], p_fcw[:, c, tap:tap + 1], facc[i_][:, 0:n], OP.mult, OP.add)
                        k.stt(facc[11 + i_][:, 0:n], at[:, 22 + c, tap:n + tap], p_fcw[:, 22 + c, tap:tap + 1], facc[11 + i_][:, 0:n], OP.mult, OP.add)
                for i_, c in enumerate(cs):
                    k.act(facc[i_][:, 0:n], facc[i_][:, 0:n], AF.Silu)
                for i_, c in enumerate(cs):
                    k.tt("pool", gg[c][:, 0:n], facc[i_][:, 0:n], facc[11 + i_][:, 0:n], OP.mult)
            for dc in range(8):
                ps = psm[dc % 2]
                for c in range(22):
                    k.mm(ps[:, 0:n], wd[:, c, dc * 128:(dc + 1) * 128], gg[c][:, 0:n], start=(c == 0), stop=(c == 21))
                k.copy("act", fs[:, dc, 0:n], ps[:, 0:n])
            rs = rms_scale(fs[:, :, 0:n], n, psr[0], ntmp)
            t_ = ntmp[2][:, :, 0:n]
            k.tt("dve", t_, fs[:, :, 0:n], rs.bmid(KC), OP.mult)
            for c in range(KC):
                k.stt(xn[:, c, 0:n], t_[:, c, :], VEC[:, l * 6 + 5, c, s:s + 1], xmt[j][:, c, 0:n], OP.mult, OP.add)
            if last:
                k.st(fmv(yout, 0, 8, t0 - L, n), xn[:, :, 0:n])
            else:
                k.st(fmv(X2, 0, 8, t0, n), xn[:, :, 0:n])
                norm_mod(xn[:, :, 0:n], n, l + 1, 0, s, psr[1], un[j][:, :, 0:n], ntmp)
                k.st(fmv(U, 0, 8, t0, n), un[j][:, :, 0:n])
        k.pers_off = pers_save
        k.barrier()
        Xcur = X2

    k.barrier()
    k.emit()
    es.close()
    return nc, k.n_instr


def _prep_common(inp, S, L, depth):
    fm, tm = _cols()
    T = L + S
    f = lambda a: np.ascontiguousarray(a, dtype=np.float32)
    pc = lambda v, c: f(v.reshape(c, 128).T)
    ident = np.eye(128, dtype=np.float32)
    ss, tt = np.meshgrid(np.arange(128), np.arange(128), indexing="ij")
    triF = (ss <= tt).astype(np.float32)
    triB = (ss >= tt).astype(np.float32)
    cf32 = f(np.stack([ident, triF, triB], axis=1).reshape(128, 384))
    p = np.arange(128)
    d = p % 64
    half, part, fi = d // 32, (d % 32) // 16, d % 16
    inv_freq = (10000.0 ** (-np.arange(16, dtype=np.float32) / 16)).astype(np.float32)
    t = np.arange(S)
    pos = np.where(half[:, None] == 0, (t // 64)[None, :], (t % 64)[None, :]).astype(np.float32)
    ang = pos * inv_freq[fi][:, None]
    cosT = np.ones((128, T), np.float32)
    sinT = np.zeros((128, T), np.float32)
    cosT[:, L:] = np.cos(ang)
    sinT[:, L:] = np.sin(ang) * np.where(part == 0, -1.0, 1.0)[:, None]
    invc = np.zeros((4, T), np.float32)
    for g, w in enumerate((2, 4, 8, 16)):
        h = w // 2
        for (o, n) in ((0, L), (L, S)):
            tt_ = np.arange(n)
            lo = np.clip(tt_ - h, 0, n)
            hi = np.clip(tt_ + h, 0, n)
            invc[g, o:o + n] = 1.0 / (hi - lo)
    invc = f(np.broadcast_to(invc.reshape(1, 4 * T), (128, 4 * T)))
    com = dict(cf32=cf32, cosT=f(cosT), sinT=f(sinT), invc=invc)
    com["w_mod"] = f(inp["w_mod"][:depth])
    com["b_mod"] = f(np.stack([pc(inp["b_mod"][l], 48) for l in range(depth)]))
    com["norm_g"] = f(np.stack([pc(inp["norm_g"][l].reshape(-1), 32) for l in range(depth)]))
    com["w_fm"] = f(inp["w_in"][:depth][:, :, fm])
    com["w_tm"] = f(inp["w_in"][:depth][:, :, tm])
    com["qkcw"] = f(np.stack([inp["mlstm_qk_conv"][l].T.reshape(8, 128, 3).transpose(1, 0, 2).reshape(128, 24) for l in range(depth)]))
    com["gbias"] = f(np.stack([np.broadcast_to(inp["mlstm_gate_bias"][l].reshape(1, 16), (128, 16)) for l in range(depth)]))
    com["poolw"] = f(np.stack([inp["pool_w"][l].transpose(1, 0, 2).reshape(128, 512) for l in range(depth)]))
    com["pscale"] = f(np.stack([pc(inp["pool_scale"][l], 4) for l in range(depth)]))
    com["sink"] = f(np.stack([np.broadcast_to(inp["attn_sink"][l].reshape(1, 8), (128, 8)) for l in range(depth)]))
    com["scw"] = f(np.stack([inp["sconv_w"][l].T.reshape(4, 128, 3).transpose(1, 0, 2).reshape(128, 12) for l in range(depth)]))
    com["w_br"] = f(inp["w_branch"][:depth].reshape(depth, 2048, D))
    com["w_out"] = f(inp["w_out"][:depth])
    com["f_up"] = f(inp["ffn_up"][:depth])
    com["f_cw"] = f(np.stack([inp["ffn_conv"][l].T.reshape(44, 128, 3).transpose(1, 0, 2).reshape(128, 132) for l in range(depth)]))
    com["f_dn"] = f(inp["ffn_down"][:depth])
    return com


def _prep_core(inp, b, com):
    f = lambda a: np.ascontiguousarray(a, dtype=np.float32)
    m = dict(com)
    m["xin"] = f(np.concatenate([inp["ctx"][b].T, inp["x"][b].T], axis=1))
    sc = np.stack([inp["c"][b].reshape(8, 128).T, inp["c_ctx"].reshape(8, 128).T], axis=2)
    m["sc_in"] = f(sc.reshape(128, 16))
    return m


def run(inp, depth, debug=False, n_cores=None):
    inp = {k_: np.asarray(v) for k_, v in inp.items()}
    B, S, _ = inp["x"].shape
    L = inp["ctx"].shape[1]
    nc, n_instr = build(S, L, depth, debug)
    com = _prep_common(inp, S, L, depth)
    ncores = n_cores or B
    in_maps = [_prep_core(inp, b % B, com) for b in range(ncores)]
    res = run_bass_kernel_spmd(nc, in_maps, core_ids=list(range(ncores)))
    return res, n_instr


def kernel(**inputs):
    res, _ = run(inputs, 4)
    B = inputs["x"].shape[0]
    return np.stack([np.ascontiguousarray(res.results[b]["yout"].T) for b in range(B)]).astype(np.float32)
```
